# Optimizing a Trainium2 kernel written in Bass

```python
import jax
import jax.numpy as jnp
from jax import lax
import numpy as np

D_MODEL = 1024
BATCH = 4
SEQ = 4096
DEPTH = 1

GRID_W = 64
CTX_LEN = 256
D_MIX = D_MODEL
HG_WIDTH = D_MIX // 2
HG_HEAD_DIM = 128
HG_HEADS = HG_WIDTH // HG_HEAD_DIM
ML_WIDTH = D_MIX - HG_WIDTH
ML_HEADS = 4
ML_V_DIM = ML_WIDTH // ML_HEADS
ML_QK_DIM = ML_V_DIM // 2
ML_QK_WIDTH = ML_HEADS * ML_QK_DIM
N_ML_GATES = 4 * ML_HEADS
IN_SIZES = (HG_WIDTH, HG_WIDTH, HG_WIDTH, HG_WIDTH, HG_WIDTH, ML_QK_WIDTH, ML_QK_WIDTH, ML_WIDTH, ML_WIDTH, N_ML_GATES)
D_IN = 5 * HG_WIDTH + 2 * ML_QK_WIDTH + 2 * ML_WIDTH + N_ML_GATES
CONV_K = 3
D_FF = 2816
CHUNK = 64
N_MOD = 9
EPS = 1e-6

kernel_name = 'hybrid_hgrn2_mlstm_macaron_dit_layer'


def rmsnorm(x, w):
    xf = x.astype(jnp.float32)
    y = xf * lax.rsqrt(jnp.mean(xf * xf, axis=-1, keepdims=True) + EPS)
    return (y * w.astype(jnp.float32)).astype(x.dtype)


def head_rmsnorm(x, w, n_heads):
    shp = x.shape
    xf = x.astype(jnp.float32).reshape(shp[:-1] + (n_heads, shp[-1] // n_heads))
    y = xf * lax.rsqrt(jnp.mean(xf * xf, axis=-1, keepdims=True) + EPS)
    return y.reshape(shp) * w.astype(jnp.float32)


def modulate(xn, shift, scale):
    return xn * (1 + scale) + shift


def swiglu(h, w1, w2):
    g, u = jnp.split(h @ w1, 2, axis=-1)
    return (jax.nn.silu(g) * u) @ w2


def split_heads(a, n_heads):
    return a.reshape(a.shape[:-1] + (n_heads, a.shape[-1] // n_heads))


def seg_reverse(a, n_ctx):
    return jnp.concatenate([jnp.flip(a[:, :n_ctx], axis=1), jnp.flip(a[:, n_ctx:], axis=1)], axis=1)


def to_chunks(a):
    bp, t, h, d = a.shape
    return a.reshape(bp, t // CHUNK, CHUNK, h, d).transpose(1, 0, 3, 2, 4)


def gates_to_chunks(g):
    bp, t, h = g.shape
    return g.reshape(bp, t // CHUNK, CHUNK, h).transpose(1, 0, 3, 2)


def from_chunks(a):
    n, bp, h, l, d = a.shape
    return a.transpose(1, 0, 3, 2, 4).reshape(bp, n * l, h, d)


def hgrn2_chunk_scan(q, k, v, log_f):
    bp, _, h, dk = q.shape
    dv = v.shape[-1]
    scan_order = jnp.tril(jnp.ones((CHUNK, CHUNK), dtype=bool))

    def step(state, inp):
        qb, kb, vb, fb = inp
        b = jnp.cumsum(fb, axis=2)
        diff = b[:, :, :, None, :] - b[:, :, None, :, :]
        decay = jnp.exp(jnp.where(scan_order[:, :, None], diff, -jnp.inf))
        scores = jnp.einsum('bhtd,bhtsd,bhsd->bhts', qb, decay, kb)
        o = jnp.einsum('bhts,bhse->bhte', scores, vb) + jnp.einsum('bhtd,bhde->bhte', qb * jnp.exp(b), state)
        b_last = b[:, :, -1:, :]
        state = jnp.exp(b_last[:, :, 0, :, None]) * state + jnp.einsum('bhsd,bhse->bhde', kb * jnp.exp(b_last - b), vb)
        return state, o

    s0 = jnp.zeros((bp, h, dk, dv), jnp.float32)
    _, out = lax.scan(step, s0, (to_chunks(q), to_chunks(k), to_chunks(v), to_chunks(log_f)))
    return from_chunks(out)


def mlstm_chunk_scan(q, k, v, i_pre, log_f):
    bp, _, h, dqk = q.shape
    dv = v.shape[-1]
    scan_order = jnp.tril(jnp.ones((CHUNK, CHUNK), dtype=bool))

    def step(carry, inp):
        c_mem, n_mem, m = carry
        qb, kb, vb, ib, fb = inp
        b = jnp.cumsum(fb, axis=-1)
        d_log = jnp.where(scan_order, b[..., :, None] - b[..., None, :] + ib[..., None, :], -jnp.inf)
        inter_log = b + m[..., None]
        m_t = jnp.maximum(jnp.max(d_log, axis=-1), inter_log)
        w = jnp.exp(d_log - m_t[..., None])
        inter_w = jnp.exp(inter_log - m_t)
        qk = jnp.einsum('bhtd,bhsd->bhts', qb, kb) * w
        num = jnp.einsum('bhts,bhse->bhte', qk, vb) + inter_w[..., None] * jnp.einsum('bhtd,bhde->bhte', qb, c_mem)
        den = jnp.sum(qk, axis=-1) + inter_w * jnp.einsum('bhtd,bhd->bht', qb, n_mem)
        h_out = num / jnp.maximum(jnp.abs(den), jnp.exp(-m_t))[..., None]
        b_last = b[..., -1]
        state_log = b_last[..., None] - b + ib
        m_new = jnp.maximum(b_last + m, jnp.max(state_log, axis=-1))
        carry_w = jnp.exp(b_last + m - m_new)
        s_w = jnp.exp(state_log - m_new[..., None])
        c_mem = carry_w[..., None, None] * c_mem + jnp.einsum('bhs,bhsd,bhse->bhde', s_w, kb, vb)
        n_mem = carry_w[..., None] * n_mem + jnp.einsum('bhs,bhsd->bhd', s_w, kb)
        return (c_mem, n_mem, m_new), h_out

    init = (jnp.zeros((bp, h, dqk, dv), jnp.float32), jnp.zeros((bp, h, dqk), jnp.float32), jnp.zeros((bp, h), jnp.float32))
    _, out = lax.scan(step, init, (to_chunks(q), to_chunks(k), to_chunks(v), gates_to_chunks(i_pre), gates_to_chunks(log_f)))
    return from_chunks(out)


def short_conv(a, w, b, n_ctx):
    w = w.astype(a.dtype)
    a_ctx, a_lat = a[:, :n_ctx], a[:, n_ctx:]
    bsz, s_lat, ch = a_lat.shape
    rows = s_lat // GRID_W
    lat = lax.conv_general_dilated(a_lat.reshape(bsz, rows, GRID_W, ch), w[:, :, None, :], (1, 1), 'SAME',
                                   dimension_numbers=('NHWC', 'HWIO', 'NHWC'), feature_group_count=ch)
    ctx = lax.conv_general_dilated(a_ctx, w[CONV_K // 2][:, None, :], (1,), 'SAME',
                                   dimension_numbers=('NWC', 'WIO', 'NWC'), feature_group_count=ch)
    return jnp.concatenate([ctx, lat.reshape(bsz, s_lat, ch)], axis=1) + b


def parallel_mixer(h_ctx, h_lat, w_in, ml_gate_b, conv_w, conv_b, lb, hg_norm_w, ml_norm_w, w_out, lat_only):
    f32 = jnp.float32
    n_ctx = h_ctx.shape[1]
    bsz = h_lat.shape[0]
    out_dtype = h_lat.dtype
    h_all = jnp.concatenate([h_ctx, h_lat], axis=1)
    proj = h_all @ w_in
    splits = [int(s) for s in np.cumsum(IN_SIZES)[:-1]]
    hg_q, hg_i, hg_g, hg_f_fwd, hg_f_bwd, ml_q, ml_k, ml_v, ml_o, ml_gates = jnp.split(proj, splits, axis=-1)

    def both_dirs(a):
        return jnp.concatenate([a, seg_reverse(a, n_ctx)], axis=0)

    def merge_dirs(o):
        return o[:bsz] + seg_reverse(o[bsz:], n_ctx)

    q = split_heads(jax.nn.silu(hg_q.astype(f32)) * HG_HEAD_DIM ** -0.5, HG_HEADS)
    v = split_heads(hg_i.astype(f32), HG_HEADS)
    lb_dir = jnp.repeat(lb, bsz, axis=0)[:, None, :]
    f_pre = jnp.concatenate([hg_f_fwd, seg_reverse(hg_f_bwd, n_ctx)], axis=0).astype(f32)
    log_f = jnp.log(lb_dir + (1 - lb_dir) * jax.nn.sigmoid(f_pre))
    k_in = (1 - lb_dir) * jax.nn.sigmoid(-f_pre)
    hg_o = hgrn2_chunk_scan(both_dirs(q), split_heads(k_in, HG_HEADS), both_dirs(v), split_heads(log_f, HG_HEADS))
    hg_o = merge_dirs(hg_o).reshape(bsz, -1, HG_WIDTH)
    hg_out = head_rmsnorm(hg_o, hg_norm_w, HG_HEADS) * jax.nn.silu(hg_g.astype(f32))

    qk = jax.nn.silu(short_conv(jnp.concatenate([ml_q, ml_k], axis=-1), conv_w, conv_b, n_ctx)).astype(f32)
    mq, mk = jnp.split(qk, 2, axis=-1)
    mq = split_heads(mq * ML_QK_DIM ** -0.5, ML_HEADS)
    mk = split_heads(mk, ML_HEADS)
    mv = split_heads(ml_v.astype(f32), ML_HEADS)
    gates = ml_gates.astype(f32) + ml_gate_b.astype(f32)
    i_fwd, f_fwd, i_bwd, f_bwd = jnp.split(gates, 4, axis=-1)
    i_dir = jnp.concatenate([i_fwd, seg_reverse(i_bwd, n_ctx)], axis=0)
    logf_dir = jax.nn.log_sigmoid(jnp.concatenate([f_fwd, seg_reverse(f_bwd, n_ctx)], axis=0))
    ml_h = mlstm_chunk_scan(both_dirs(mq), both_dirs(mk), both_dirs(mv), i_dir, logf_dir)
    ml_h = merge_dirs(ml_h).reshape(bsz, -1, ML_WIDTH)
    ml_out = jax.nn.sigmoid(ml_o.astype(f32)) * head_rmsnorm(ml_h, ml_norm_w, ML_HEADS)

    merged = jnp.concatenate([hg_out, ml_out], axis=-1).astype(out_dtype)
    lat_out = merged[:, n_ctx:] @ w_out
    ctx_out = None if lat_only else merged[:, :n_ctx] @ w_out
    return ctx_out, lat_out


def setup_inputs(seed: int = 0) -> dict:
    key = jax.random.key(seed)
    ks = jax.random.split(key, 26)
    nrm = jax.random.normal
    f32 = jnp.float32

    def gain(k, shape):
        return 1.0 + 0.01 * nrm(k, shape, f32)

    def fbias(k):
        return 3.0 + 3.0 * jax.random.uniform(k, (DEPTH, ML_HEADS), f32)

    ml_gate_b = jnp.concatenate([0.1 * nrm(ks[10], (DEPTH, ML_HEADS), f32), fbias(ks[11]),
                                 0.1 * nrm(ks[12], (DEPTH, ML_HEADS), f32), fbias(ks[13])], axis=-1)
    return {
        'x': nrm(ks[0], (BATCH, SEQ, D_MODEL), f32),
        'c': nrm(ks[1], (BATCH, D_MODEL), f32),
        'ctx': nrm(ks[2], (BATCH, CTX_LEN, D_MODEL), f32),
        'c_ctx': nrm(ks[3], (D_MODEL,), f32),
        'w_mod': nrm(ks[4], (DEPTH, D_MODEL, N_MOD * D_MODEL), f32) * D_MODEL ** -0.5,
        'b_mod': 0.01 * nrm(ks[5], (DEPTH, N_MOD * D_MODEL), f32),
        'norm1_w': gain(ks[6], (DEPTH, D_MODEL)),
        'ffn1_w1': nrm(ks[7], (DEPTH, D_MODEL, 2 * D_FF), f32) * D_MODEL ** -0.5,
        'ffn1_w2': nrm(ks[8], (DEPTH, D_FF, D_MODEL), f32) * D_FF ** -0.5,
        'norm2_w': gain(ks[9], (DEPTH, D_MODEL)),
        'w_in': nrm(ks[14], (DEPTH, D_MODEL, D_IN), f32) * D_MODEL ** -0.5,
        'ml_gate_b': ml_gate_b,
        'ml_conv_w': nrm(ks[15], (DEPTH, CONV_K, CONV_K, 2 * ML_QK_WIDTH), f32) / CONV_K,
        'ml_conv_b': 0.01 * nrm(ks[16], (DEPTH, 2 * ML_QK_WIDTH), f32),
        'hg_lb_logits': 0.5 * nrm(ks[17], (DEPTH + 1, 2, HG_WIDTH), f32),
        'hg_norm_w': gain(ks[18], (DEPTH, HG_WIDTH)),
        'ml_norm_w': gain(ks[19], (DEPTH, ML_WIDTH)),
        'w_out': nrm(ks[20], (DEPTH, D_MIX, D_MODEL), f32) * D_MIX ** -0.5,
        'norm3_w': gain(ks[21], (DEPTH, D_MODEL)),
        'ffn2_w1': nrm(ks[22], (DEPTH, D_MODEL, 2 * D_FF), f32) * D_MODEL ** -0.5,
        'ffn2_w2': nrm(ks[23], (DEPTH, D_FF, D_MODEL), f32) * D_FF ** -0.5,
        'final_norm_w': gain(ks[24], (D_MODEL,)),
    }


def reference(x, c, ctx, c_ctx, w_mod, b_mod, norm1_w, ffn1_w1, ffn1_w2, norm2_w, w_in, ml_gate_b,
              ml_conv_w, ml_conv_b, hg_lb_logits, hg_norm_w, ml_norm_w, w_out, norm3_w, ffn2_w1, ffn2_w2,
              final_norm_w):
    lb_all = jnp.cumsum(jax.nn.softmax(hg_lb_logits.astype(jnp.float32), axis=0), axis=0)
    s_ctx = ctx

    def ffn_sub(s, mods, nw, w1, w2):
        shift, scale, gate = mods
        return s + 0.5 * gate * swiglu(modulate(rmsnorm(s, nw), shift, scale), w1, w2)

    for l in range(DEPTH):
        last = l == DEPTH - 1
        mod_lat = jnp.split((jax.nn.silu(c) @ w_mod[l] + b_mod[l])[:, None, :], N_MOD, axis=-1)
        mod_ctx = jnp.split(jax.nn.silu(c_ctx) @ w_mod[l] + b_mod[l], N_MOD, axis=-1)

        x = ffn_sub(x, mod_lat[0:3], norm1_w[l], ffn1_w1[l], ffn1_w2[l])
        s_ctx = ffn_sub(s_ctx, mod_ctx[0:3], norm1_w[l], ffn1_w1[l], ffn1_w2[l])

        h_lat = modulate(rmsnorm(x, norm2_w[l]), mod_lat[3], mod_lat[4])
        h_ctx = modulate(rmsnorm(s_ctx, norm2_w[l]), mod_ctx[3], mod_ctx[4])
        ctx_mix, lat_mix = parallel_mixer(h_ctx, h_lat, w_in[l], ml_gate_b[l], ml_conv_w[l], ml_conv_b[l],
                                          lb_all[l], hg_norm_w[l], ml_norm_w[l], w_out[l], last)
        x = x + mod_lat[5] * lat_mix

        x = ffn_sub(x, mod_lat[6:9], norm3_w[l], ffn2_w1[l], ffn2_w2[l])
        if not last:
            s_ctx = s_ctx + mod_ctx[5] * ctx_mix
            s_ctx = ffn_sub(s_ctx, mod_ctx[6:9], norm3_w[l], ffn2_w1[l], ffn2_w2[l])

    return rmsnorm(x, final_norm_w)
```

```python
import numpy as np
import concourse.bass as bass
import concourse.mybir as mybir
from concourse.bass_utils import run_bass_kernel_spmd

F32 = mybir.dt.float32
BF16 = mybir.dt.bfloat16
AF = mybir.ActivationFunctionType
ALU = mybir.AluOpType

D = 1024
NDC = 8
SEQ = 4096
NCTX = 256
HALF = 2048
HALO = 64
NTOK = NCTX + HALF + HALO
DFF = 2816
NJ = 22
EPS = 1e-6
NCORES = 8
NSEM_DMA = 8

T_F1W1 = 0
T_F1W2 = 11
T_WIN = 19
T_WOUT = 29
T_F2W1 = 31
T_F2W2 = 42
NTILES = 50
G_HF1, G_HF2, G_HV, G_HQ, G_HG, G_MQK, G_MV, G_MO, G_MG1, G_MG2 = range(10)

V_N1, V_N2, V_N3, V_NF = 0, 8, 16, 24
V_BMOD = 32
V_HNW = 104
V_MNW = 108
V_CW = 112
V_CB = 148
V_SEL = 152
V_C = 160
NV = 176
R_LB = 0
R_GB = 2048
NR = 3072
C_ID = 0
C_TRI1 = 128
C_TRI2 = 256
C_TRC1 = 384
C_TRC2 = 512
NCONST = 640


class Buf:
    __slots__ = ("name", "w", "r")

    def __init__(self, name):
        self.name = name
        self.w = None
        self.r = []


class Op:
    __slots__ = ("eng", "fn", "deps", "is_dma", "signal", "count", "sem", "target", "idx", "cc")


ENGS = ("pe", "act", "dve", "pool", "sp")
SAME_ENGINE_SYNC = True


class Sched:
    def __init__(self):
        self.ops = {e: [] for e in ENGS}
        self.n = 0
        self.frozen = False
        self.in_hook = False
        self.hook = None

    def add(self, eng, fn, R=(), W=(), dma=False, extra=(), cc=False):
        op = Op()
        if OPLIMIT and self.n >= OPLIMIT and not self.frozen and not self.in_hook:
            self.in_hook = True
            self.hook()
            self.in_hook = False
            self.frozen = True
        if self.frozen:
            op.eng, op.is_dma, op.signal, op.deps = eng, dma, False, []
            return op
        op.cc = cc
        op.eng, op.fn, op.is_dma, op.signal = eng, fn, dma, False
        op.count = op.sem = op.target = None
        op.idx = self.n
        self.n += 1
        deps = list(extra)
        for b in R:
            if b.w is not None:
                deps.append(b.w)
        for b in W:
            if b.w is not None:
                deps.append(b.w)
            deps.extend(b.r)
        for b in W:
            b.w = op
            b.r = []
        for b in R:
            b.r.append(op)
        seen = set()
        op.deps = []
        for d in deps:
            if d is op or id(d) in seen:
                continue
            seen.add(id(d))
            if d.is_dma:
                op.deps.append(d)
            elif d.eng != eng:
                d.signal = True
                op.deps.append(d)
            elif SAME_ENGINE_SYNC and eng != "pe":
                d.signal = True
                op.deps.append(d)
        self.ops[eng].append(op)
        return op

    def finalize(self, nsem_dma):
        for e in ENGS:
            c = 0
            for op in self.ops[e]:
                if op.signal and not op.is_dma:
                    c += 1
                    op.count = c
        self.dma_uses = {}
        for e in ENGS:
            k = 0
            for op in self.ops[e]:
                if op.cc:
                    self.dma_uses[("cc", 0)] = 1
                    op.sem = ("cc", 0)
                    op.target = 1
                elif op.is_dma:
                    slot = k % nsem_dma
                    k += 1
                    key = (e, slot)
                    n = self.dma_uses.get(key, 0) + 1
                    self.dma_uses[key] = n
                    op.sem = key
                    op.target = 16 * n

    def emit(self, nc, block, esem, dsem):
        engobj = {"pe": "tensor", "act": "scalar", "dve": "vector", "pool": "gpsimd", "sp": "sync"}

        def make(ename):
            def body(eng):
                waited = {}

                def wait(key, sem, val):
                    if waited.get(key, 0) >= val:
                        return
                    waited[key] = val
                    eng.wait_ge(sem, val)

                for op in self.ops[ename]:
                    for d in op.deps:
                        if d.is_dma:
                            wait(("d",) + d.sem, dsem[d.sem], d.target)
                        else:
                            wait(("e", d.eng), esem[d.eng], d.count)
                    if op.is_dma and not op.cc and op.target > 16:
                        wait(("d",) + op.sem, dsem[op.sem], op.target - 16)
                    ins = op.fn(eng)
                    if op.cc:
                        ins.then_inc(dsem[op.sem])
                    elif op.is_dma:
                        ins.then_inc(dsem[op.sem], 16)
                    elif op.signal:
                        ins.then_inc(esem[ename], 1)
            return body

        for e in ENGS:
            if self.ops[e]:
                getattr(block, engobj[e])(make(e))


def build_program():
    nc = bass.Bass("TRN2", target_bir_lowering=False)
    S = Sched()

    xin = nc.dram_tensor("xin", [NTOK, D], F32, kind="ExternalInput").ap()
    wsrcs = [nc.dram_tensor(f"wsrc{i}", [10, 128, 4096], F32, kind="ExternalInput").ap() for i in range(5)]
    wmods = [nc.dram_tensor(f"wmod{i}", [4, 128, 8 * 768], F32, kind="ExternalInput").ap() for i in range(3)]
    vecs_d = nc.dram_tensor("vecs", [128, NV], F32, kind="ExternalInput").ap()
    rows_d = nc.dram_tensor("rows", [1, NR], F32, kind="ExternalInput").ap()
    consts_d = nc.dram_tensor("consts", [128, NCONST], F32, kind="ExternalInput").ap()
    yout = nc.dram_tensor("yout", [HALF, D], F32, kind="ExternalOutput").ap()
    wbf = nc.dram_tensor("wbf", [NTILES, 128, 4096], BF16)
    cc_in = nc.dram_tensor("cc_in", [128, 1024], F32)
    cc_out = nc.dram_tensor("cc_out", [NCORES * 128, 1024], F32)

    ARENA_F32 = 52800
    ctx = nc.sbuf_tensor("arena", [128, ARENA_F32], F32)
    arena = ctx.__enter__()
    psum_ctx = [nc.psum_tensor(f"pb{i}", [128, 512], F32) for i in range(8)]
    PB = [c.__enter__() for c in psum_ctx]
    PBUF = [Buf(f"psum{i}") for i in range(8)]

    class Alloc:
        def __init__(self):
            self.top = 0
            self.marks = []

        def f32(self, name, *shape):
            n = int(np.prod(shape))
            a = arena[:, self.top:self.top + n]
            self.top += n + (n & 1)
            assert self.top <= ARENA_F32, (name, self.top)
            if len(shape) == 2:
                a = a.rearrange("p (a b) -> p a b", b=shape[1])
            elif len(shape) == 3:
                a = a.rearrange("p (a b c) -> p a b c", b=shape[1], c=shape[2])
            return a

        def bf16(self, name, *shape):
            n = int(np.prod(shape))
            nw = (n + 1) // 2
            nw += nw & 1
            a = arena[:, self.top:self.top + nw].bitcast(BF16)[:, 0:n]
            self.top += nw
            assert self.top <= ARENA_F32, (name, self.top)
            if len(shape) == 2:
                a = a.rearrange("p (a b) -> p a b", b=shape[1])
            elif len(shape) == 3:
                a = a.rearrange("p (a b c) -> p a b c", b=shape[1], c=shape[2])
            return a

        def mark(self):
            self.marks.append(self.top)

        def release(self):
            self.top = self.marks.pop()

    A = Alloc()
    allbufs = []

    def B(name):
        b = Buf(name)
        allbufs.append(b)
        return b

    def barrier():
        firsts = []
        dmas = []
        for e in ENGS:
            dl = [o for o in S.ops[e] if o.is_dma]
            dmas.extend(dl[-NSEM_DMA:])
        sig = {
            "pe": lambda eng: eng.matmul(PB[7][0:1, 0:2], ones_b[:, 0:1], ones_b[:, 0:2], start=True, stop=True),
            "act": lambda eng: eng.activation(bscr[:, 0:1], bscr[:, 4:5], AF.Copy),
            "dve": lambda eng: eng.memset(bscr[:, 1:2], 0.0),
            "pool": lambda eng: eng.memset(bscr[:, 2:3], 0.0),
        }
        for e in ("pe", "act", "dve", "pool"):
            op = S.add(e, sig[e], R=[b_bscr] if e == "act" else [], W=[b_bar[e]])
            op.signal = True
            firsts.append(op)
        for e in ENGS:
            S.add(e, lambda eng: eng.nop(), extra=[o for o in firsts if o.eng != e] + dmas)
        for b in allbufs + PBUF:
            b.w = None
            b.r = []

    S.hook = barrier
    def mm(out, lhsT, rhs, start, stop, R, W):
        S.add("pe", lambda e: e.matmul(out, lhsT, rhs, start=start, stop=stop), R=R, W=W)

    def tr(out, in_, ident, R, W):
        S.add("pe", lambda e: e.transpose(out, in_, ident), R=R, W=W)

    def act(out, in_, func, R, W, bias=0.0, scale=1.0):
        S.add("act", lambda e: e.activation(out, in_, func, bias=bias, scale=scale), R=R, W=W)

    def tt(out, in0, in1, op, R, W, eng="dve"):
        S.add(eng, lambda e: e.tensor_tensor(out, in0, in1, op), R=R, W=W)

    def ts(out, in0, s1, s2, op0, op1, R, W, eng="dve"):
        if op1 is None:
            S.add(eng, lambda e: e.tensor_scalar(out, in0, s1, None, op0), R=R, W=W)
        else:
            S.add(eng, lambda e: e.tensor_scalar(out, in0, s1, s2, op0, op1), R=R, W=W)

    def stt(out, in0, sc, in1, op0, op1, R, W):
        S.add("dve", lambda e: e.scalar_tensor_tensor(out, in0, sc, in1, op0, op1), R=R, W=W)

    def cp(out, in_, R, W, eng="dve"):
        S.add(eng, lambda e: e.tensor_copy(out, in_), R=R, W=W)

    def recip(out, in_, R, W):
        S.add("dve", lambda e: e.reciprocal(out, in_), R=R, W=W)

    def memset(ap, val, W, eng="dve"):
        S.add(eng, lambda e: e.memset(ap, val), R=[], W=W)

    def dma(eng, out, in_, R, W):
        S.add(eng, lambda e: e.dma_start(out=out, in_=in_), R=R, W=W, dma=True)

    pstate = {"i": 0}

    def psum():
        i = pstate["i"] % 5
        pstate["i"] += 1
        return PB[i], PBUF[i]

    xs = nc.dram_tensor("xs", [128, NDC, HALF], F32).ap()
    b_xs = [B(f"xs{i}") for i in range(HALF // 256)]
    ofd = nc.dram_tensor("ofd", [128, 8, HALF], F32).ap()
    b_ofd = [B(f"ofd{i}") for i in range(HALF // 128)]
    hT = A.bf16("hT", NDC, NTOK)
    NHB = (NTOK + 127) // 128
    b_hT = [B(f"hT{i}") for i in range(NHB + 1)]

    def hbufs(t0, n):
        return [b_hT[i] for i in range(t0 // 128, (t0 + n - 1) // 128 + 1)]

    bscr = A.f32("bscr", 8)
    b_bscr = Buf("bscr")
    b_bar = {e: Buf("bar_" + e) for e in ENGS}
    vecs = A.f32("vecs", NV)
    b_vecs = B("vecs")
    consts = A.f32("consts", NCONST)
    b_consts = B("consts")
    constb = A.bf16("constb", NCONST)
    b_constb = B("constb")
    ones_b = A.bf16("ones_b", 256)
    b_ones = B("ones")
    modT = A.f32("modT", 72, 2)
    b_mod = B("mod")
    modc = A.f32("modc", 2, 40)
    b_modc = B("modc")
    epst = A.f32("epst", 4)
    b_eps = B("eps")
    lnb = A.f32("lnb", 2)
    NSLOT = 3
    wpool = [A.bf16(f"wp{i}", 4096) for i in range(NSLOT)]
    b_wpool = [B(f"wp{i}") for i in range(NSLOT)]
    b_wbf = [B(f"wbf{i}") for i in range(NTILES)]
    wst = {"i": 0}

    def wload(tile, ncols=4096):
        i = wst["i"] % NSLOT
        wst["i"] += 1
        dma("sp", wpool[i][:, 0:ncols], wbf[tile, :, 0:ncols], R=[b_wbf[tile]], W=[b_wpool[i]])
        return wpool[i], b_wpool[i]

    dma("sp", vecs, vecs_d, R=[], W=[b_vecs])
    dma("sp", consts, consts_d, R=[], W=[b_consts])
    cp(constb, consts, R=[b_consts], W=[b_constb])
    memset(ones_b, 1.0, W=[b_ones])
    memset(bscr, 0.0, W=[b_bscr])
    memset(epst[:, 0:1], EPS, W=[b_eps])
    memset(lnb[:, 0:1], 0.5 * float(np.log(128.0)), W=[b_eps])
    ident_f = consts[:, C_ID:C_ID + 128]
    ident_b = constb[:, C_ID:C_ID + 128]

    A.mark()
    cb = A.bf16("cb", 16, 2)
    b_cb = B("cb")
    ctmp = A.f32("ctmp", 16)
    b_ctmp = B("ctmp")
    act(ctmp, vecs[:, V_C:V_C + 16], AF.Exp, R=[b_vecs], W=[b_ctmp], scale=-1.0)
    ts(ctmp, ctmp, 1.0, None, ALU.add, None, R=[b_ctmp], W=[b_ctmp])
    recip(ctmp, ctmp, R=[b_ctmp], W=[b_ctmp])
    tt(cb[:, 0:8, 0], ctmp[:, 0:8], vecs[:, V_C:V_C + 8], ALU.mult, R=[b_ctmp, b_vecs], W=[b_cb])
    tt(cb[:, 0:8, 1], ctmp[:, 8:16], vecs[:, V_C + 8:V_C + 16], ALU.mult, R=[b_ctmp, b_vecs], W=[b_cb])
    wm = [A.bf16(f"wm{i}", 8, 768) for i in range(2)]
    b_wm = [B(f"wm{i}") for i in range(2)]
    pm, bpm = psum()
    for blk in range(12):
        w, bw = wm[blk % 2], b_wm[blk % 2]
        dma("pool", w, wmods[blk // 4][blk % 4].rearrange("p (k c) -> p k c", c=768), R=[], W=[bw])
        for mi in range(6):
            m = blk * 6 + mi
            for kc in range(8):
                mm(pm[:, 2 * m:2 * m + 2], w[:, kc, mi * 128:(mi + 1) * 128], cb[:, kc, :],
                   start=(kc == 0), stop=(kc == 7), R=[bw, b_cb], W=[bpm])
    pm3 = pm[:, 0:144].rearrange("p (m t) -> p m t", t=2)
    for t in range(2):
        tt(modT[:, :, t], pm3[:, :, t], vecs[:, V_BMOD:V_BMOD + 72], ALU.add, R=[bpm, b_vecs], W=[b_mod])
    barrier()
    A.release()

    for t in range(NTILES):
        dma("pool", wbf[t], wsrcs[t // 10][t % 10], R=[], W=[b_wbf[t]])

    def mod_ap(k, which):
        return modT[:, 8 * k:8 * k + 8, which]

    modc2 = A.f32("modc2", 2, 32)
    b_modc2 = B("modc2")
    for which in range(2):
        for (dst, kshift, kscale, nwc) in ((modc[:, which, 0:8], 0, 1, V_N1), (modc[:, which, 24:32], 3, 4, V_N2),
                                           (modc2[:, which, 0:8], 6, 7, V_N3)):
            bb = b_modc if dst is not modc2[:, which, 0:8] else b_modc2
            ts(dst, mod_ap(kscale, which), 1.0, 32.0, ALU.add, ALU.mult, R=[b_mod], W=[b_modc, b_modc2])
            tt(dst, dst, vecs[:, nwc:nwc + 8], ALU.mult, R=[b_vecs, b_modc, b_modc2], W=[b_modc, b_modc2])
        cp(modc[:, which, 8:16], mod_ap(0, which), R=[b_mod], W=[b_modc, b_modc2])
        ts(modc[:, which, 16:24], mod_ap(2, which), 0.5, None, ALU.mult, None, R=[b_mod], W=[b_modc, b_modc2])
        cp(modc[:, which, 32:40], mod_ap(3, which), R=[b_mod], W=[b_modc, b_modc2])
        cp(modc2[:, which, 8:16], mod_ap(6, which), R=[b_mod], W=[b_modc, b_modc2])
        ts(modc2[:, which, 16:24], mod_ap(8, which), 0.5, None, ALU.mult, None, R=[b_mod], W=[b_modc, b_modc2])
        cp(modc2[:, which, 24:32], mod_ap(5, which), R=[b_mod], W=[b_modc, b_modc2])
    afin = A.f32("afin", 8)
    b_afin = B("afin")
    ts(afin, vecs[:, V_NF:V_NF + 8], 32.0, None, ALU.mult, None, R=[b_vecs], W=[b_afin])
    b_mc = [b_modc, b_modc2]

    def norm_stats(xblk, bx, n, tmp_sq, b_sq, rs, b_rs):
        ps, bps = psum()
        for dc in range(NDC):
            k = dc % 2
            act(tmp_sq[k][:, 0:n], xblk[:, dc, 0:n], AF.Square, R=bx, W=[b_sq[k]])
            mm(ps[:, 0:n], ones_b[:, 0:128], tmp_sq[k][:, 0:n], start=(dc == 0), stop=(dc == 7),
               R=[b_sq[k], b_ones], W=[bps])
        act(rs[:, 0:n], ps[:, 0:n], AF.Ln, R=[bps, b_eps], W=[b_rs], bias=1024.0 * EPS)
        act(rs[:, 0:n], rs[:, 0:n], AF.Exp, R=[b_rs], W=[b_rs], scale=-0.5)

    def norm_apply(xblk, bx, n, rs, b_rs, tmp, b_tmp, a_ap, b_ap, out, bout):
        for dc in range(NDC):
            k = dc % 2
            tt(tmp[k][:, 0:n], xblk[:, dc, 0:n], rs[:, 0:n], ALU.mult, R=bx + [b_rs], W=[b_tmp[k]])
            act(out[:, dc, 0:n], tmp[k][:, 0:n], AF.Identity, R=[b_tmp[k]] + b_mc, W=bout,
                bias=b_ap[:, dc:dc + 1], scale=a_ap[:, dc:dc + 1])

    def ffn(xblk, bx, n, hin, bhin, w1_tile0, w2_tile0, gate_ap, fb):
        actb, b_actb, sg, b_sg = fb
        for jb in range(11):
            w, bw = wload(w1_tile0 + jb)
            w3 = w.rearrange("p (k c) -> p k c", c=512)
            for sub in range(2):
                j = 2 * jb + sub
                pg, bpg = psum()
                pu, bpu = psum()
                for kc in range(8):
                    mm(pg[:, 0:n], w3[:, kc, sub * 256:sub * 256 + 128], hin[:, kc, 0:n],
                       start=(kc == 0), stop=(kc == 7), R=[bw] + bhin, W=[bpg])
                for kc in range(8):
                    mm(pu[:, 0:n], w3[:, kc, sub * 256 + 128:sub * 256 + 256], hin[:, kc, 0:n],
                       start=(kc == 0), stop=(kc == 7), R=[bw] + bhin, W=[bpu])
                k = j % 2
                act(sg[k][:, 0:n], pg[:, 0:n], AF.Silu, R=[bpg], W=[b_sg[k]])
                tt(actb[:, j, 0:n], sg[k][:, 0:n], pu[:, 0:n], ALU.mult, R=[b_sg[k], bpu], W=[b_actb[j]])
        for dc in range(NDC):
            w, bw = wload(w2_tile0 + dc, ncols=NJ * 128)
            w3 = w[:, 0:NJ * 128].rearrange("p (k c) -> p k c", c=128)
            po, bpo = psum()
            for kc in range(NJ):
                mm(po[:, 0:n], w3[:, kc, :], actb[:, kc, 0:n], start=(kc == 0), stop=(kc == NJ - 1),
                   R=[bw, b_actb[kc]], W=[bpo])
            stt(xblk[:, dc, 0:n], po[:, 0:n], gate_ap[:, dc:dc + 1], xblk[:, dc, 0:n], ALU.mult, ALU.add,
                R=[bpo] + bx + b_mc, W=bx)

    def load_xT(dst, bdst, row0, n, stg, b_stg):
        nt = (n + 127) // 128
        pts = [psum() for _ in range(NDC)] if False else None
        for ti in range(nt):
            r = min(128, n - ti * 128)
            k = ti % 2
            dma("sp", stg[k][0:r, :], xin[row0 + ti * 128: row0 + ti * 128 + r, :], R=[], W=[b_stg[k]])
            for half in range(2):
                pt, bpt = psum()
                for q in range(4):
                    dc = half * 4 + q
                    tr(pt[:, q * 128:q * 128 + r], stg[k][0:r, dc * 128:(dc + 1) * 128], ident_f[0:r, 0:r],
                       R=[b_stg[k], b_consts], W=[bpt])
                src = pt[:, 0:512].rearrange("p (q t) -> p q t", t=128)[:, :, 0:r]
                if half == 0:
                    cp(dst[:, 0:4, ti * 128:ti * 128 + r], src, R=[bpt], W=bdst)
                else:
                    S.add("act", lambda e, o=dst[:, 4:8, ti * 128:ti * 128 + r], i=src: e.copy(o, i),
                          R=[bpt], W=bdst)

    A.mark()
    stg = [A.f32(f"stg{i}", D) for i in range(2)]
    b_stg = [B(f"stg{i}") for i in range(2)]
    h1 = [A.bf16(f"h1_{i}", NDC, 512) for i in range(2)]
    b_h1 = [B(f"h1_{i}") for i in range(2)]
    actb = A.bf16("actb", NJ, 512)
    b_actb = [B(f"actb{j}") for j in range(NJ)]
    sg = [A.f32(f"sg{i}", 512) for i in range(2)]
    b_sg = [B(f"sg{i}") for i in range(2)]
    tmpn = [A.f32(f"tmpn{i}", 512) for i in range(2)]
    b_tmpn = [B(f"tmpn{i}") for i in range(2)]
    sqb = [A.bf16(f"sqb{i}", 512) for i in range(2)]
    b_sqb = [B(f"sqb{i}") for i in range(2)]
    rs = A.f32("rs", 512)
    b_rs = B("rs")
    xbk = [A.f32(f"xbk{i}", NDC, 512) for i in range(2)]
    b_xbk = [B(f"xbk{i}") for i in range(2)]
    fb = (actb, b_actb, sg, b_sg)

    blocks = [(0, NCTX, 1)] + [(NCTX + 512 * i, 512, 0) for i in range(4)] + [(NCTX + HALF, HALO, 0)]
    for bi, (t0, n, which) in enumerate(blocks):
        xblk, bx = xbk[bi % 2], [b_xbk[bi % 2]]
        load_xT(xblk, bx, t0, n, stg, b_stg)
        hh, bh = h1[bi % 2], [b_h1[bi % 2]]
        norm_stats(xblk, bx, n, sqb, b_sqb, rs, b_rs)
        norm_apply(xblk, bx, n, rs, b_rs, tmpn, b_tmpn, modc[:, which, 0:8], modc[:, which, 8:16], hh, bh)
        ffn(xblk, bx, n, hh, bh, T_F1W1, T_F1W2, modc[:, which, 16:24], fb)
        norm_stats(xblk, bx, n, sqb, b_sqb, rs, b_rs)
        norm_apply(xblk, bx, n, rs, b_rs, tmpn, b_tmpn, modc[:, which, 24:32], modc[:, which, 32:40],
                   hT[:, :, t0:t0 + n], hbufs(t0, n))
        if which == 0 and n == 512:
            l0 = t0 - NCTX
            dma("sp", xs[:, :, l0:l0 + 512], xblk, R=bx, W=[b_xs[l0 // 256], b_xs[l0 // 256 + 1]])
    barrier()
    A.release()
    if DEBUG_STOP == 1:
        S.frozen = True

    A.mark()
    gbias = A.f32("gbias", 1024)
    b_rows = B("rows")
    dma("sp", gbias, rows_d[:, R_GB:R_GB + 1024].partition_broadcast(128), R=[], W=[b_rows])
    lbb = A.f32("lbb", 2, 512)
    omlb = A.f32("omlb", 2, 512)
    b_lb = B("lb")
    A.mark()
    rows = A.f32("rows", 2048)
    b_rowl = B("rowl")
    dma("sp", rows, rows_d[:, R_LB:R_LB + 2048].partition_broadcast(128), R=[], W=[b_rowl])
    for dr in range(2):
        a0 = rows[:, R_LB + dr * 1024: R_LB + dr * 1024 + 512]
        a1 = rows[:, R_LB + dr * 1024 + 512: R_LB + dr * 1024 + 1024]
        tt(lbb[:, dr, :], a1, a0, ALU.subtract, R=[b_rowl], W=[b_lb])
        act(lbb[:, dr, :], lbb[:, dr, :], AF.Exp, R=[b_lb], W=[b_lb])
        ts(lbb[:, dr, :], lbb[:, dr, :], 1.0, None, ALU.add, None, R=[b_lb], W=[b_lb])
        recip(lbb[:, dr, :], lbb[:, dr, :], R=[b_lb], W=[b_lb])
        ts(omlb[:, dr, :], lbb[:, dr, :], -1.0, 1.0, ALU.mult, ALU.add, R=[b_lb], W=[b_lb])
    barrier()
    A.release()

    ofs = [A.f32(f"ofs{i}", 8, 128) for i in range(2)]
    b_ofs = [B(f"ofs{i}") for i in range(2)]
    xg = [A.f32(f"xg{i}", NDC, 256) for i in range(2)]
    b_xg = [B(f"xg{i}") for i in range(2)]
    ofq = {"i": 0, "x": 0}
    MQT = A.bf16("MQT", 2, NCTX + HALF)
    MKT = A.bf16("MKT", 2, NCTX + HALF)
    b_MQK = [B(f"MQK{i}") for i in range(18)]
    S_hg = A.f32("S_hg", 4, 128)
    S_hgb = A.bf16("S_hgb", 4, 128)
    S_ml = A.f32("S_ml", 2, 256)
    S_mlb = A.bf16("S_z", 4 * 256).rearrange("p (a b c) -> p a b c", a=2, b=2, c=256)
    b_Shg, b_Shgb, b_Sml, b_Smlb = B("Shg"), B("Shgb"), B("Sml"), B("Smlb")
    memset(S_hg, 0.0, W=[b_Shg])
    memset(S_hgb, 0.0, W=[b_Shgb])
    memset(S_ml, 0.0, W=[b_Sml])
    memset(S_mlb, 0.0, W=[b_Smlb])
    maskb = A.bf16("maskb", 2, 4, 128)
    b_mask = B("mask")
    for p in range(2):
        for h in range(4):
            cp(maskb[:, p, h, :], consts[:, C_TRI1 + 128 * p: C_TRI1 + 128 * p + 128], R=[b_consts], W=[b_mask])
    ntri = A.f32("ntri", 2, 128)
    ntrc = A.f32("ntrc", 2, 128)
    b_ntri = B("ntri")
    for p in range(2):
        ts(ntri[:, p, :], consts[:, C_TRI1 + 128 * p:C_TRI1 + 128 * p + 128], -1.0, None, ALU.mult, None,
           R=[b_consts], W=[b_ntri])
        ts(ntrc[:, p, :], consts[:, C_TRC1 + 128 * p:C_TRC1 + 128 * p + 128], -1.0, None, ALU.mult, None,
           R=[b_consts], W=[b_ntri])

    if DEBUG_STOP == 11:
        barrier()
        S.frozen = True
    A.mark()
    GR = 8
    qkpre = A.f32("qkpre", 4, (GR + 2) * 64)
    b_qkpre = B("qkpre")
    cacc = A.f32("cacc", 4, GR * 64)
    b_cacc = B("cacc")
    etmp = A.f32("etmp", 4, GR * 64)
    b_etmp = B("etmp")
    wqk, b_wqk = wload(T_WIN + G_MQK)
    wqk3 = wqk.rearrange("p (k c) -> p k c", c=512)

    def proj_fm(dst_fn, w3, bw, c0, t0, n, extraW):
        ps, bps = psum()
        for kc in range(8):
            mm(ps[:, 0:n], w3[:, kc, c0:c0 + 128], hT[:, kc, t0:t0 + n], start=(kc == 0), stop=(kc == 7),
               R=[bw] + hbufs(t0, n), W=[bps])
        return ps, bps

    def conv_taps(is_ctx):
        taps = []
        for dy in range(3):
            for dx in range(3):
                if is_ctx and dy != 1:
                    continue
                taps.append((dy, dx))
        return taps

    for g in range(5):
        is_ctx = (g == 0)
        if is_ctx:
            nrows, tbase = 4, 0
        else:
            nrows, tbase = GR, NCTX + (g - 1) * 512
        for c in range(4):
            if is_ctx:
                ps, bps = proj_fm(None, wqk3, b_wqk, c * 128, 0, 256, None)
                S.add("act", lambda e, o=qkpre[:, c, 0:256], i=ps[:, 0:256]: e.copy(o, i), R=[bps], W=[b_qkpre])
            else:
                lo = tbase - 64 if g > 1 else tbase
                hi = tbase + 512 + 64
                n = hi - lo
                off = 0 if g > 1 else 64
                if g == 1:
                    memset(qkpre[:, c, 0:64], 0.0, W=[b_qkpre], eng="pool")
                ps, bps = proj_fm(None, wqk3, b_wqk, c * 128, lo, 512, None)
                S.add("act", lambda e, o=qkpre[:, c, off:off + 512], i=ps[:, 0:512]: e.copy(o, i),
                      R=[bps], W=[b_qkpre])
                ps2, bps2 = proj_fm(None, wqk3, b_wqk, c * 128, lo + 512, n - 512, None)
                S.add("act", lambda e, o=qkpre[:, c, off + 512:off + n], i=ps2[:, 0:n - 512]: e.copy(o, i),
                      R=[bps2], W=[b_qkpre])
        for c in range(4):
            if is_ctx:
                wc = vecs[:, V_CW + (1 * 3 + 1) * 4 + c: V_CW + (1 * 3 + 1) * 4 + c + 1]
                ts(cacc[:, c, 0:256], qkpre[:, c, 0:256], wc, vecs[:, V_CB + c:V_CB + c + 1], ALU.mult, ALU.add,
                   R=[b_qkpre, b_vecs], W=[b_cacc])
                for dx in (0, 2):
                    wap = vecs[:, V_CW + (3 + dx) * 4 + c: V_CW + (3 + dx) * 4 + c + 1]
                    sh = dx - 1
                    o_lo, o_hi = max(0, -sh), min(256, 256 - sh)
                    stt(cacc[:, c, o_lo:o_hi], qkpre[:, c, o_lo + sh:o_hi + sh], wap, cacc[:, c, o_lo:o_hi],
                        ALU.mult, ALU.add, R=[b_qkpre, b_vecs, b_cacc], W=[b_cacc])
                ntk = 256
            else:
                pre3 = qkpre[:, c, :].rearrange("p (r w) -> p r w", w=64)
                acc3 = cacc[:, c, :].rearrange("p (r w) -> p r w", w=64)
                wc = vecs[:, V_CW + 4 * 4 + c: V_CW + 4 * 4 + c + 1]
                ts(cacc[:, c, :], qkpre[:, c, 64:64 + 512], wc, vecs[:, V_CB + c:V_CB + c + 1], ALU.mult, ALU.add,
                   R=[b_qkpre, b_vecs], W=[b_cacc])
                for dy in range(3):
                    for dx in range(3):
                        if dy == 1 and dx == 1:
                            continue
                        wap = vecs[:, V_CW + (dy * 3 + dx) * 4 + c: V_CW + (dy * 3 + dx) * 4 + c + 1]
                        sh = dx - 1
                        o_lo, o_hi = max(0, -sh), min(64, 64 - sh)
                        stt(acc3[:, :, o_lo:o_hi], pre3[:, dy:dy + GR, o_lo + sh:o_hi + sh], wap,
                            acc3[:, :, o_lo:o_hi], ALU.mult, ALU.add, R=[b_qkpre, b_vecs, b_cacc], W=[b_cacc])
                ntk = 512
            act(etmp[:, c, 0:ntk], cacc[:, c, 0:ntk], AF.Exp, R=[b_cacc], W=[b_etmp], scale=-1.0)
            ts(etmp[:, c, 0:ntk], etmp[:, c, 0:ntk], 1.0, None, ALU.add, None, R=[b_etmp], W=[b_etmp])
            recip(etmp[:, c, 0:ntk], etmp[:, c, 0:ntk], R=[b_etmp], W=[b_etmp])
            lt0 = tbase
            wb_ = [b_MQK[i] for i in range(lt0 // 128, (lt0 + ntk) // 128)]
            if c < 2:
                stt(MQT[:, c, lt0:lt0 + ntk], cacc[:, c, 0:ntk], 0.125, etmp[:, c, 0:ntk], ALU.mult, ALU.mult,
                    R=[b_cacc, b_etmp], W=wb_)
            else:
                tt(MKT[:, c - 2, lt0:lt0 + ntk], cacc[:, c, 0:ntk], etmp[:, c, 0:ntk], ALU.mult,
                   R=[b_cacc, b_etmp], W=wb_)
    barrier()
    A.release()

    if DEBUG_STOP == 12:
        S.frozen = True
    NB = 2
    GT = NB * 128
    LF = A.f32("LF", NB, 512)
    KK = A.f32("KK", NB, 512)
    b_LF = [B(f"LF{i}") for i in range(NB)]
    b_KK = [B(f"KK{i}") for i in range(NB)]
    KH = A.bf16("KH", NB, 512)
    b_KH = [B(f"KH{i}") for i in range(NB)]
    KT = A.bf16("KT", 4, GT)
    b_KT = [B(f"KT{i}") for i in range(NB)]
    VV = A.bf16("VV", NB, 512)
    b_VV = [B(f"VV{i}") for i in range(NB)]
    EBT = A.f32("EBT", 4, GT)
    b_EBT = [B(f"EBT{i}") for i in range(NB)]
    QT = A.bf16("QT", 4, GT)
    b_QT = B("QT")
    OS = A.f32("OS", 8, GT)
    b_OS = [B(f"OS{i}") for i in range(NB)]
    MRG = A.bf16("MRG", 8, GT)
    b_MRG = B("MRG")
    LFN = A.f32("LFN", NB, 256)
    GI = A.f32("GI", NB, 256)
    b_LFN = [B(f"LFN{i}") for i in range(NB)]
    MKH = A.bf16("MKH", NB, 256)
    b_MKH = [B(f"MKH{i}") for i in range(NB)]
    MV = A.bf16("MV", NB, 4, 256)
    b_MV = [B(f"MV{i}") for i in range(NB)]
    memset(MV, 1.0, W=b_MV)
    EBM = A.f32("EBM", 2, GT)
    b_EBM = [B(f"EBM{i}") for i in range(NB)]
    MKt = A.bf16("MKtz", 4 * GT).rearrange("p (a b c) -> p a b c", a=2, b=2, c=GT)
    MQt = A.bf16("MQt", 2, GT)
    b_MKt = [B(f"MKt{i}") for i in range(NB)]
    b_MQt = [B(f"MQt{i}") for i in range(NB)]
    memset(MKt, 0.0, W=b_MKt)

    t512 = [A.f32(f"t512_{i}", 512) for i in range(4)]
    b_t512 = [B(f"t512_{i}") for i in range(4)]
    tb512 = [A.bf16(f"tb512_{i}", 512) for i in range(3)]
    b_tb512 = [B(f"tb512_{i}") for i in range(3)]
    SM = [A.bf16(f"SM{i}", 4, 128) for i in range(2)]
    b_SM = [B(f"SM{i}") for i in range(2)]
    tq = {"f": 0, "b": 0, "s": 0}

    def tmpf():
        i = tq["f"] % 4
        tq["f"] += 1
        return t512[i], b_t512[i]

    def tmpb():
        i = tq["b"] % 3
        tq["b"] += 1
        return tb512[i], b_tb512[i]

    def proj_tm(w3, bw, t0, ncols=512, c0=0):
        ps, bps = psum()
        for kc in range(8):
            mm(ps[:, 0:ncols], hT[:, kc, t0:t0 + 128], w3[:, kc, c0:c0 + ncols], start=(kc == 0), stop=(kc == 7),
               R=[bw] + hbufs(t0, 128), W=[bps])
        return ps, bps

    def sigmoid_into(out, bout, src, bsrc, n):
        act(out, src, AF.Exp, R=bsrc, W=bout, scale=-1.0)
        ts(out, out, 1.0, None, ALU.add, None, R=bout, W=bout)
        recip(out, out, R=bout, W=bout)

    def mixer_group(p, t0, is_ctx):
        lat0 = t0 - NCTX
        tri_f = consts[:, C_TRI1 + 128 * p: C_TRI1 + 128 * p + 128]
        trc_f = consts[:, C_TRC1 + 128 * p: C_TRC1 + 128 * p + 128]
        ntri_f = ntri[:, p, :]
        ntrc_f = ntrc[:, p, :]
        blks = list(range(NB)) if p == 0 else list(range(NB - 1, -1, -1))
        chunk_order = (0, 1) if p == 0 else (1, 0)
        last_pos = (lambda c: c * 64 + 63) if p == 0 else (lambda c: c * 64)

        w, bw = wload(T_WIN + (G_HF1 if p == 0 else G_HF2))
        w3 = w.rearrange("p (k c) -> p k c", c=512)
        for b in range(NB):
            ps, bps = proj_tm(w3, bw, t0 + b * 128)
            sgm, bsg = tmpf()
            sigmoid_into(sgm, [bsg], ps[:, 0:512], [bps], 512)
            tt(sgm, sgm, omlb[:, p, :], ALU.mult, R=[bsg, b_lb], W=[bsg])
            tt(sgm, sgm, lbb[:, p, :], ALU.add, R=[bsg, b_lb], W=[bsg])
            act(LF[:, b, :], sgm, AF.Ln, R=[bsg], W=[b_LF[b]])
            ts(KK[:, b, :], sgm, -1.0, 1.0, ALU.mult, ALU.add, R=[bsg], W=[b_KK[b]])
        for b in range(NB):
            pb_, bpb = psum()
            mm(pb_[:, 0:512], tri_f, LF[:, b, :], True, True, R=[b_consts, b_LF[b]], W=[bpb])
            pr_, bpr = psum()
            mm(pr_[:, 0:512], trc_f, LF[:, b, :], True, True, R=[b_consts, b_LF[b]], W=[bpr])
            e1, be1 = tmpf()
            act(e1, pb_[:, 0:512], AF.Exp, R=[bpb], W=[be1], scale=-1.0)
            kt, bkt = tmpb()
            tt(kt, KK[:, b, :], e1, ALU.mult, R=[b_KK[b], be1], W=[bkt])
            e2, be2 = tmpf()
            act(e2, pr_[:, 0:512], AF.Exp, R=[bpr], W=[be2])
            tt(KH[:, b, :], KK[:, b, :], e2, ALU.mult, R=[b_KK[b], be2], W=[b_KH[b]])
            if not is_ctx:
                ptb, bptb = psum()
                ptb16 = ptb[:, 0:256].bitcast(BF16)
                for h in range(4):
                    tr(ptb16[:, h * 128:(h + 1) * 128], kt[:, h * 128:(h + 1) * 128], ident_b,
                       R=[bkt, b_constb], W=[bptb])
                S.add("act", lambda e, o=KT[:, :, b * 128:(b + 1) * 128],
                      i=ptb16.rearrange("p (h t) -> p h t", t=128): e.copy(o, i), R=[bptb], W=[b_KT[b]])
            pbt, bpbt = psum()
            for h in range(4):
                mm(pbt[:, h * 128:(h + 1) * 128], LF[:, b, h * 128:(h + 1) * 128], tri_f, True, True,
                   R=[b_consts, b_LF[b]], W=[bpbt])
            act(EBT[:, :, b * 128:(b + 1) * 128], pbt[:, 0:512].rearrange("p (h t) -> p h t", t=128), AF.Exp,
                R=[bpbt], W=[b_EBT[b]])
        if MG_STOP == 1 and not is_ctx:
            barrier()
            S.frozen = True
        w, bw = wload(T_WIN + G_HV)
        w3 = w.rearrange("p (k c) -> p k c", c=512)
        for b in range(NB):
            ps, bps = proj_tm(w3, bw, t0 + b * 128)
            S.add("act", lambda e, o=VV[:, b, :], i=ps[:, 0:512]: e.copy(o, i), R=[bps], W=[b_VV[b]])
        if not is_ctx:
            w, bw = wload(T_WIN + G_HQ)
            w3 = w.rearrange("p (k c) -> p k c", c=512)
            for h in range(4):
                ps, bps = proj_fm(None, w3, bw, h * 128, t0, GT, None)
                sq_, bsq = tmpf()
                sigmoid_into(sq_[:, 0:GT], [bsq], ps[:, 0:GT], [bps], GT)
                tt(sq_[:, 0:GT], sq_[:, 0:GT], ps[:, 0:GT], ALU.mult, R=[bsq, bps], W=[bsq])
                stt(QT[:, h, :], sq_[:, 0:GT], 128.0 ** -0.5, EBT[:, h, :], ALU.mult, ALU.mult,
                    R=[bsq] + b_EBT, W=[b_QT])

        if MG_STOP == 2 and not is_ctx:
            barrier()
            S.frozen = True
        w, bw = wload(T_WIN + (G_MG1 if p == 0 else G_MG2))
        w3 = w.rearrange("p (k c) -> p k c", c=512)
        gb = gbias[:, p * 512: p * 512 + 512]
        for b in range(NB):
            ps, bps = proj_tm(w3, bw, t0 + b * 128)
            tt(GI[:, b, :], ps[:, 0:256], gb[:, 0:256], ALU.add, R=[bps, b_rows], W=[b_LFN[b]])
            tf_, btf = tmpf()
            tt(tf_[:, 0:256], ps[:, 256:512], gb[:, 256:512], ALU.add, R=[bps, b_rows], W=[btf])
            act(tf_[:, 0:256], tf_[:, 0:256], AF.Exp, R=[btf], W=[btf], scale=-1.0)
            act(LFN[:, b, :], tf_[:, 0:256], AF.Ln, R=[btf], W=[b_LFN[b]], bias=1.0)
        for b in range(NB):
            tb0 = t0 + b * 128
            pr_, bpr = psum()
            mm(pr_[:, 0:256], ntrc_f, LFN[:, b, :], True, False, R=[b_ntri, b_LFN[b]], W=[bpr])
            mm(pr_[:, 0:256], ident_f, GI[:, b, :], False, True, R=[b_consts, b_LFN[b]], W=[bpr])
            e2, be2 = tmpf()
            act(e2[:, 0:256], pr_[:, 0:256], AF.Exp, R=[bpr], W=[be2])
            ptk, bptk = psum()
            ptk16 = ptk[:, 0:128].bitcast(BF16)
            for pr2 in range(2):
                tr(ptk16[:, pr2 * 128:(pr2 + 1) * 128], MKT[:, pr2, tb0:tb0 + 128], ident_b,
                   R=[b_MQK[tb0 // 128], b_constb], W=[bptk])
            tt(MKH[:, b, :], ptk16[:, 0:256], e2[:, 0:256], ALU.mult, R=[bptk, be2], W=[b_MKH[b]])
            pf, bpf = psum()
            for pr2 in range(2):
                mm(pf[:, pr2 * 128:(pr2 + 1) * 128], LFN[:, b, pr2 * 128:(pr2 + 1) * 128], ntri_f, True, True,
                   R=[b_ntri, b_LFN[b]], W=[bpf])
                mm(pf[:, 256 + pr2 * 128:256 + (pr2 + 1) * 128], LFN[:, b, pr2 * 128:(pr2 + 1) * 128], tri_f,
                   True, False, R=[b_consts, b_LFN[b]], W=[bpf])
                mm(pf[:, 256 + pr2 * 128:256 + (pr2 + 1) * 128], GI[:, b, pr2 * 128:(pr2 + 1) * 128], ident_f,
                   False, True, R=[b_consts, b_LFN[b]], W=[bpf])
            act(EBM[:, :, b * 128:(b + 1) * 128], pf[:, 0:256].rearrange("p (h t) -> p h t", t=128), AF.Exp,
                R=[bpf], W=[b_EBM[b]])
            if not is_ctx:
                e3, be3 = tmpf()
                act(e3[:, 0:256], pf[:, 256:512], AF.Exp, R=[bpf], W=[be3])
                e33 = e3[:, 0:256].rearrange("p (h t) -> p h t", t=128)
                for hf in range(2):
                    hp = slice(hf * 64, (hf + 1) * 64)
                    tt(MKt[hp, :, hf, b * 128:(b + 1) * 128], MKT[hp, :, tb0:tb0 + 128], e33[hp, :, :], ALU.mult,
                       R=[b_MQK[tb0 // 128], be3], W=[b_MKt[b]])
                tt(MQt[:, :, b * 128:(b + 1) * 128], MQT[:, :, tb0:tb0 + 128], EBM[:, :, b * 128:(b + 1) * 128],
                   ALU.mult, R=[b_MQK[tb0 // 128], b_EBM[b]], W=[b_MQt[b]])
        w, bw = wload(T_WIN + G_MV)
        w3 = w.rearrange("p (k c) -> p k c", c=512)
        for b in range(NB):
            ps, bps = proj_tm(w3, bw, t0 + b * 128)
            S.add("act", lambda e, o=MV[:, b, :, 0:128], i=ps[:, 0:512].rearrange("p (h c) -> p h c", c=128):
                  e.copy(o, i), R=[bps], W=[b_MV[b]])

        if not is_ctx:
            MARKS.setdefault("mg3", S.n)
        if MG_STOP == 3 and not is_ctx:
            barrier()
            S.frozen = True
        for b in blks:
            bs = slice(b * 128, (b + 1) * 128)
            if not is_ctx:
                pss, bpss = psum()
                for h in range(4):
                    mm(pss[:, h * 128:(h + 1) * 128], KT[:, h, bs], QT[:, h, bs], True, True,
                       R=[b_KT[b], b_QT], W=[bpss])
                sm, bsm = SM[0], b_SM[0]
                tt(sm, pss[:, 0:512].rearrange("p (h t) -> p h t", t=128), maskb[:, p, :, :], ALU.mult,
                   R=[bpss, b_mask], W=[bsm])
                po, bpo = PB[5], PBUF[5]
                psm, bpsm = psum()
                for h in range(4):
                    pr2, hf = h // 2, h % 2
                    mm(psm[:, h * 128:(h + 1) * 128], MKt[:, pr2, hf, bs],
                       MQt[:, pr2, bs], True, True, R=[b_MKt[b], b_MQt[b]], W=[bpsm])
                sm2, bsm2 = SM[1], b_SM[1]
                tt(sm2, psm[:, 0:512].rearrange("p (h t) -> p h t", t=128), maskb[:, p, :, :], ALU.mult,
                   R=[bpsm, b_mask], W=[bsm2])
                pn, bpn = PB[6], PBUF[6]
                pd, bpd = PB[7], PBUF[7]
            for ci, c in enumerate(chunk_order):
                cs = slice(b * 128 + c * 64, b * 128 + c * 64 + 64)
                rows_c = slice(c * 64, c * 64 + 64)
                lp = b * 128 + last_pos(c)
                if not is_ctx:
                    cc_ = slice(c * 64, c * 64 + 64)
                    for h in range(4):
                        oc = slice(h * 128 + c * 64, h * 128 + c * 64 + 64)
                        mm(po[:, oc], VV[rows_c, b, h * 128:(h + 1) * 128], sm[rows_c, h, cc_], True, False,
                           R=[b_VV[b], bsm], W=[bpo])
                        mm(po[:, oc], S_hgb[:, h, :], QT[:, h, cs], False, True, R=[b_Shgb, b_QT], W=[bpo])
                    for h in range(4):
                        pr2, hf = h // 2, h % 2
                        hp_ = slice(hf * 64, (hf + 1) * 64)
                        oc = slice(h * 128 + c * 64, h * 128 + c * 64 + 64)
                        mm(pn[:, oc], MV[rows_c, b, h, 0:128], sm2[rows_c, h, cc_], True, False,
                           R=[b_MV[b], bsm2], W=[bpn])
                        mm(pn[:, oc], S_mlb[:, pr2, hf, 0:128], MQt[:, pr2, cs], False, True,
                           R=[b_Smlb, b_MQt[b]], W=[bpn])
                        mm(pd[:, oc], MV[rows_c, b, h, 128:256], sm2[rows_c, h, cc_], True, False,
                           R=[b_MV[b], bsm2], W=[bpd])
                        mm(pd[:, oc], S_mlb[:, pr2, hf, 128:256], MQt[:, pr2, cs], False, True,
                           R=[b_Smlb, b_MQt[b]], W=[bpd])
                pu, bpu = psum()
                for h in range(4):
                    mm(pu[:, h * 128:(h + 1) * 128], KH[rows_c, b, h * 128:(h + 1) * 128],
                       VV[rows_c, b, h * 128:(h + 1) * 128], True, True, R=[b_KH[b], b_VV[b]], W=[bpu])
                for h in range(4):
                    stt(S_hg[:, h, :], S_hg[:, h, :], EBT[:, h, lp:lp + 1], pu[:, h * 128:(h + 1) * 128],
                        ALU.mult, ALU.add, R=[b_Shg, b_EBT[b], bpu], W=[b_Shg])
                S.add("act", lambda e: e.copy(S_hgb, S_hg), R=[b_Shg], W=[b_Shgb])
                for pr2 in range(2):
                    pum, bpum = psum()
                    for hf in range(2):
                        h = pr2 * 2 + hf
                        mm(pum[:, hf * 256:(hf + 1) * 256], MKH[rows_c, b, pr2 * 128:(pr2 + 1) * 128],
                           MV[rows_c, b, h, :], True, True, R=[b_MKH[b], b_MV[b]], W=[bpum])
                    for hf in range(2):
                        hp = slice(hf * 64, (hf + 1) * 64)
                        stt(S_ml[hp, pr2, :], S_ml[hp, pr2, :], EBM[hp, pr2, lp:lp + 1],
                            pum[hp, hf * 256:(hf + 1) * 256], ALU.mult, ALU.add,
                            R=[b_Sml, b_EBM[b], bpum], W=[b_Sml])
                for hf in range(2):
                    hp = slice(hf * 64, (hf + 1) * 64)
                    S.add("act", lambda e, o=S_mlb[hp, :, hf, :], i=S_ml[hp, :, :]: e.copy(o, i), R=[b_Sml], W=[b_Smlb])
            if is_ctx:
                continue
            MARKS.setdefault("mg5", S.n)
            if MG_STOP == 5:
                barrier()
                S.frozen = True
            lb0 = lat0 + b * 128
            ofb = [b_ofd[lb0 // 128]]
            po3 = po[:, 0:512].rearrange("p (h t) -> p h t", t=128)
            pn3 = pn[:, 0:512].rearrange("p (h t) -> p h t", t=128)
            rr, brr = tmpf()
            act(rr, pd[:, 0:512], AF.Abs, R=[bpd], W=[brr])
            ts(rr, rr, 1.0, None, ALU.max, None, R=[brr], W=[brr])
            recip(rr, rr, R=[brr], W=[brr])
            rr3 = rr.rearrange("p (h t) -> p h t", t=128)
            k = ofq["i"] % 2
            ofq["i"] += 1
            st_, bst = ofs[k], b_ofs[k]
            if p == 0:
                S.add("act", lambda e, o=st_[:, 0:4, :], i=po3: e.copy(o, i), R=[bpo], W=[bst])
                tt(st_[:, 4:8, :], pn3, rr3, ALU.mult, R=[bpn, brr], W=[bst])
                dma("sp", ofd[:, :, lb0:lb0 + 128], st_, R=[bst], W=ofb)
            else:
                dma("sp", st_, ofd[:, :, lb0:lb0 + 128], R=ofb, W=[bst])
                tt(OS[:, 0:4, bs], po3, st_[:, 0:4, :], ALU.add, R=[bpo, bst], W=[b_OS[b]])
                tt(rr3, pn3, rr3, ALU.mult, R=[bpn, brr], W=[brr])
                tt(OS[:, 4:8, bs], rr3, st_[:, 4:8, :], ALU.add, R=[brr, bst], W=[b_OS[b]])

        if p == 0 or is_ctx:
            return
        for hh in range(8):
            sqv, bsqv = tmpb()
            act(sqv[:, 0:GT], OS[:, hh, :], AF.Square, R=b_OS, W=[bsqv])
            pss, bpss = psum()
            mm(pss[:, 0:GT], ones_b[:, 0:128], sqv[:, 0:GT], True, True, R=[b_ones, bsqv], W=[bpss])
            rs_, brs = tmpf()
            act(rs_[:, 0:GT], pss[:, 0:GT], AF.Ln, R=[bpss], W=[brs], bias=128.0 * EPS)
            act(rs_[:, 0:GT], rs_[:, 0:GT], AF.Exp, R=[brs], W=[brs], scale=-0.5,
                bias=lnb[:, 0:1])
            tt(OS[:, hh, :], OS[:, hh, :], rs_[:, 0:GT], ALU.mult, R=b_OS + [brs], W=b_OS)
        for grp, gtile in ((0, G_HG), (1, G_MO)):
            w, bw = wload(T_WIN + gtile)
            w3 = w.rearrange("p (k c) -> p k c", c=512)
            for h in range(4):
                ps, bps = proj_fm(None, w3, bw, h * 128, t0, GT, None)
                sq_, bsq = tmpf()
                sigmoid_into(sq_[:, 0:GT], [bsq], ps[:, 0:GT], [bps], GT)
                if grp == 0:
                    tt(sq_[:, 0:GT], sq_[:, 0:GT], ps[:, 0:GT], ALU.mult, R=[bsq, bps], W=[bsq])
                nwap = vecs[:, (V_HNW if grp == 0 else V_MNW) + h:(V_HNW if grp == 0 else V_MNW) + h + 1]
                stt(MRG[:, grp * 4 + h, :], OS[:, grp * 4 + h, :], nwap, sq_[:, 0:GT], ALU.mult, ALU.mult,
                    R=b_OS + [bsq, b_vecs], W=[b_MRG])
        k = ofq["x"] % 2
        ofq["x"] += 1
        xgb, bxg = xg[k], b_xg[k]
        xsb = [b_xs[lat0 // 256]]
        dma("sp", xgb, xs[:, :, lat0:lat0 + GT], R=xsb, W=[bxg])
        for half in range(2):
            w, bw = wload(T_WOUT + half)
            w3 = w.rearrange("p (k c) -> p k c", c=512)
            for q in range(4):
                dc = half * 4 + q
                ps, bps = psum()
                for kc in range(8):
                    mm(ps[:, 0:GT], w3[:, kc, q * 128:(q + 1) * 128], MRG[:, kc, :], kc == 0, kc == 7,
                       R=[bw, b_MRG], W=[bps])
                stt(xgb[:, dc, :], ps[:, 0:GT], modc2[:, 0, 24 + dc:24 + dc + 1],
                    xgb[:, dc, :], ALU.mult, ALU.add, R=[bps, bxg] + b_mc, W=[bxg])
        dma("sp", xs[:, :, lat0:lat0 + GT], xgb, R=[bxg], W=xsb)

    mixer_group(0, 0, True)
    if DEBUG_STOP == 13:
        barrier()
        S.frozen = True
    for g in range(HALF // GT):
        mixer_group(0, NCTX + g * GT, False)
        if DEBUG_STOP == 14 and g == 0:
            barrier()
            S.frozen = True

    if DEBUG_STOP == 2:
        barrier()
        S.frozen = True
    A.mark()
    xst = [A.f32(f"xst{i}", 1024) for i in range(2)]
    b_xst = [B(f"xst{i}") for i in range(2)]
    b_ccin, b_ccout = B("ccin"), B("ccout")
    cp(xst[0][:, 0:512], S_hg.rearrange("p h e -> p (h e)"), R=[b_Shg], W=[b_xst[0]])
    cp(xst[0][:, 512:1024], S_ml.rearrange("p h e -> p (h e)"), R=[b_Sml], W=[b_xst[0]])
    dma("pool", cc_in[:, :], xst[0], R=[b_xst[0]], W=[b_ccin])
    S.add("pool", lambda e: e.collective_compute("AllGather", ALU.bypass, replica_groups=[list(range(NCORES))],
                                                  ins=[cc_in.ap().opt()], outs=[cc_out.ap().opt()]),
          R=[b_ccin], W=[b_ccout], dma=True, cc=True)
    memset(S_hg, 0.0, W=[b_Shg])
    memset(S_ml, 0.0, W=[b_Sml])
    Shg_flat = S_hg.rearrange("p h e -> p (h e)")
    Sml_flat = S_ml.rearrange("p h e -> p (h e)")
    for r in range(NCORES):
        k = r % 2
        dma("sp", xst[k], cc_out[r * 128:(r + 1) * 128, :], R=[b_ccout], W=[b_xst[k]])
        stt(Shg_flat, xst[k][:, 0:512], vecs[:, V_SEL + r:V_SEL + r + 1], Shg_flat, ALU.mult, ALU.add,
            R=[b_xst[k], b_vecs, b_Shg], W=[b_Shg])
        stt(Sml_flat, xst[k][:, 512:1024], vecs[:, V_SEL + r:V_SEL + r + 1], Sml_flat, ALU.mult, ALU.add,
            R=[b_xst[k], b_vecs, b_Sml], W=[b_Sml])
    S.add("act", lambda e: e.copy(S_hgb, S_hg), R=[b_Shg], W=[b_Shgb])
    for hf in range(2):
        hp = slice(hf * 64, (hf + 1) * 64)
        S.add("act", lambda e, o=S_mlb[hp, :, hf, :], i=S_ml[hp, :, :]: e.copy(o, i), R=[b_Sml], W=[b_Smlb])
    A.release()

    for g in range(HALF // GT - 1, -1, -1):
        mixer_group(1, NCTX + g * GT, False)
    barrier()
    A.release()

    if DEBUG_STOP == 3:
        S.frozen = True
    A.mark()
    h1 = [A.bf16(f"h3_{i}", NDC, 512) for i in range(2)]
    b_h1 = [B(f"h3_{i}") for i in range(2)]
    actb = A.bf16("actb2", NJ, 512)
    b_actb = [B(f"actb2_{j}") for j in range(NJ)]
    sg = [A.f32(f"sg2_{i}", 512) for i in range(2)]
    b_sg = [B(f"sg2_{i}") for i in range(2)]
    tmpn = [A.f32(f"tmpn2_{i}", 512) for i in range(2)]
    b_tmpn = [B(f"tmpn2_{i}") for i in range(2)]
    sqb = [A.bf16(f"sqb2_{i}", 512) for i in range(2)]
    b_sqb = [B(f"sqb2_{i}") for i in range(2)]
    rs = A.f32("rs2", 512)
    b_rs = B("rs2")
    yT = A.f32("yT", NDC, 512)
    b_yT = B("yT")
    ost = [A.f32(f"ost{i}", D) for i in range(2)]
    b_ost = [B(f"ost{i}") for i in range(2)]
    b_y = B("yout")
    fb = (actb, b_actb, sg, b_sg)
    xbk = [A.f32(f"xbk3_{i}", NDC, 512) for i in range(2)]
    b_xbk = [B(f"xbk3_{i}") for i in range(2)]
    for i in range(4):
        xblk, bx = xbk[i % 2], [b_xbk[i % 2]]
        dma("sp", xblk, xs[:, :, i * 512:(i + 1) * 512], R=[b_xs[2 * i], b_xs[2 * i + 1]], W=bx)
        hh, bh = h1[i % 2], [b_h1[i % 2]]
        norm_stats(xblk, bx, 512, sqb, b_sqb, rs, b_rs)
        norm_apply(xblk, bx, 512, rs, b_rs, tmpn, b_tmpn, modc2[:, 0, 0:8], modc2[:, 0, 8:16], hh, bh)
        ffn(xblk, bx, 512, hh, bh, T_F2W1, T_F2W2, modc2[:, 0, 16:24], fb)
        norm_stats(xblk, bx, 512, sqb, b_sqb, rs, b_rs)
        for dc in range(NDC):
            stt(yT[:, dc, :], xblk[:, dc, :], afin[:, dc:dc + 1], rs, ALU.mult, ALU.mult,
                R=bx + [b_rs, b_afin], W=[b_yT])
        for ti in range(4):
            k = ti % 2
            for half in range(2):
                pt, bpt = psum()
                for q in range(4):
                    dc = half * 4 + q
                    tr(pt[:, q * 128:(q + 1) * 128], yT[:, dc, ti * 128:(ti + 1) * 128], ident_f,
                       R=[b_yT, b_consts], W=[bpt])
                if half == 0:
                    cp(ost[k][:, 0:512], pt[:, 0:512], R=[bpt], W=[b_ost[k]])
                else:
                    S.add("act", lambda e, o=ost[k][:, 512:1024], i_=pt[:, 0:512]: e.copy(o, i_),
                          R=[bpt], W=[b_ost[k]])
            r0 = i * 512 + ti * 128
            dma("sp", yout[r0:r0 + 128, :], ost[k], R=[b_ost[k]], W=[b_y])
    S.add("sp", lambda e: e.nop(), R=[b_y], W=[])
    A.release()

    S.finalize(NSEM_DMA)
    build_program.stats = {e: len(S.ops[e]) for e in ENGS}
    import contextlib
    with contextlib.ExitStack() as es:
        esem = {e: es.enter_context(nc.semaphore(f"s_{e}")) for e in ENGS}
        dsem = {}
        for (e, slot) in S.dma_uses:
            dsem[(e, slot)] = es.enter_context(nc.semaphore(f"d_{e}{slot}"))
        block = es.enter_context(nc.Block())
        S.emit(nc, block, esem, dsem)
    for c in reversed(psum_ctx):
        c.__exit__(None, None, None)
    ctx.__exit__(None, None, None)
    return nc


def _tile_k(w, ncols_pad=4096):
    K, C = w.shape
    t = w.reshape(K // 128, 128, C).transpose(1, 0, 2).reshape(128, (K // 128) * C)
    if t.shape[1] < ncols_pad:
        t = np.concatenate([t, np.zeros((128, ncols_pad - t.shape[1]), np.float32)], axis=1)
    return t


def _ffn_tiles(w1, w2):
    tiles = []
    for jb in range(11):
        cols = []
        for sub in range(2):
            j = 2 * jb + sub
            cols.append(w1[:, j * 128:(j + 1) * 128])
            cols.append(w1[:, DFF + j * 128: DFF + (j + 1) * 128])
        tiles.append(_tile_k(np.concatenate(cols, axis=1)))
    for dc in range(8):
        tiles.append(_tile_k(w2[:, dc * 128:(dc + 1) * 128]))
    return tiles


def _pp(v, n):
    return np.ascontiguousarray(v.reshape(n, 128).T)


_PROGRAM = None
_PREP_ONLY = False
DEBUG_STOP = 0
MG_STOP = 0
OPLIMIT = 0
MARKS = {}


def kernel(x, c, ctx, c_ctx, w_mod, b_mod, norm1_w, ffn1_w1, ffn1_w2, norm2_w, w_in, ml_gate_b,
           ml_conv_w, ml_conv_b, hg_lb_logits, hg_norm_w, ml_norm_w, w_out, norm3_w, ffn2_w1, ffn2_w2,
           final_norm_w):
    global _PROGRAM
    f = lambda a: np.asarray(a, dtype=np.float32)
    x, c, ctx, c_ctx = f(x), f(c), f(ctx), f(c_ctx)
    w_mod, b_mod = f(w_mod)[0], f(b_mod)[0]
    w_in0 = f(w_in)[0]
    gate_b = f(ml_gate_b)[0]
    conv_w, conv_b = f(ml_conv_w)[0], f(ml_conv_b)[0]
    lbl = f(hg_lb_logits)

    f1 = _ffn_tiles(f(ffn1_w1)[0], f(ffn1_w2)[0])
    f2 = _ffn_tiles(f(ffn2_w1)[0], f(ffn2_w2)[0])
    wo = f(w_out)[0]
    wo_tiles = [_tile_k(wo[:, 0:512]), _tile_k(wo[:, 512:1024])]
    wmod_t = np.stack([_tile_k(w_mod[:, blk * 768:(blk + 1) * 768], 8 * 768) for blk in range(12)])

    consts = np.zeros((128, NCONST), np.float32)
    consts[:, C_ID:C_ID + 128] = np.eye(128, dtype=np.float32)
    s = np.arange(128)[:, None]
    t = np.arange(128)[None, :]
    same = (s // 64) == (t // 64)
    consts[:, C_TRI1:C_TRI1 + 128] = (same & (s <= t))
    consts[:, C_TRI2:C_TRI2 + 128] = (same & (s >= t))
    consts[:, C_TRC1:C_TRC1 + 128] = (same & (s > t))
    consts[:, C_TRC2:C_TRC2 + 128] = (same & (s < t))

    def win_tiles(rev):
        cs = lambda a, b: w_in0[:, a:b]
        hf_f, hf_b = cs(1536, 2048), cs(2048, 2560)
        g = w_in0[:, 4096:4112]
        rep = lambda cols: np.repeat(cols, 64, axis=1)
        gf = np.concatenate([rep(g[:, 0:4]), rep(g[:, 4:8])], axis=1)
        gbw = np.concatenate([rep(g[:, 8:12]), rep(g[:, 12:16])], axis=1)
        if rev:
            hf_f, hf_b = hf_b, hf_f
            gf, gbw = gbw, gf
        groups = [hf_f, hf_b, cs(512, 1024), cs(0, 512), cs(1024, 1536), cs(2560, 3072), cs(3072, 3584),
                  cs(3584, 4096), gf, gbw]
        return [_tile_k(np.ascontiguousarray(gm)) for gm in groups]

    in_maps = []
    for core in range(NCORES):
        b, half = core // 2, core % 2
        rev = (half == 1)
        if not rev:
            toks = np.concatenate([ctx[b], x[b, 0:HALF], x[b, HALF:HALF + HALO]], axis=0)
        else:
            toks = np.concatenate([ctx[b][::-1], x[b, HALF:SEQ][::-1], x[b, HALF - HALO:HALF][::-1]], axis=0)
        tiles = f1 + win_tiles(rev) + wo_tiles + f2
        wsrc = np.stack(tiles)
        vecs = np.zeros((128, NV), np.float32)
        vecs[:, V_N1:V_N1 + 8] = _pp(f(norm1_w)[0], 8)
        vecs[:, V_N2:V_N2 + 8] = _pp(f(norm2_w)[0], 8)
        vecs[:, V_N3:V_N3 + 8] = _pp(f(norm3_w)[0], 8)
        vecs[:, V_NF:V_NF + 8] = _pp(f(final_norm_w), 8)
        vecs[:, V_BMOD:V_BMOD + 72] = _pp(b_mod, 72)
        vecs[:, V_HNW:V_HNW + 4] = _pp(f(hg_norm_w)[0], 4)
        vecs[:, V_MNW:V_MNW + 4] = _pp(f(ml_norm_w)[0], 4)
        cw = conv_w[::-1, ::-1, :] if rev else conv_w
        for dy in range(3):
            for dx in range(3):
                vecs[:, V_CW + (dy * 3 + dx) * 4: V_CW + (dy * 3 + dx) * 4 + 4] = _pp(cw[dy, dx], 4)
        vecs[:, V_CB:V_CB + 4] = _pp(conv_b, 4)
        vecs[:, V_SEL + (core ^ 1)] = 1.0
        vecs[:, V_C:V_C + 8] = _pp(c[b], 8)
        vecs[:, V_C + 8:V_C + 16] = _pp(c_ctx, 8)
        rows = np.zeros((1, NR), np.float32)
        d0, d1 = (1, 0) if rev else (0, 1)
        rows[0, R_LB:R_LB + 2048] = np.concatenate([lbl[0, d0], lbl[1, d0], lbl[0, d1], lbl[1, d1]])
        rep = lambda v: np.repeat(v, 64)
        gi_f, gf_f, gi_b, gf_b = gate_b[0:4], gate_b[4:8], gate_b[8:12], gate_b[12:16]
        if rev:
            gi_f, gf_f, gi_b, gf_b = gi_b, gf_b, gi_f, gf_f
        rows[0, R_GB:R_GB + 1024] = np.concatenate([rep(gi_f), rep(gf_f), rep(gi_b), rep(gf_b)])
        m = {"xin": np.ascontiguousarray(toks), "vecs": vecs, "rows": rows, "consts": consts}
        for i in range(5):
            m[f"wsrc{i}"] = np.ascontiguousarray(wsrc[10 * i:10 * i + 10])
        for i in range(3):
            m[f"wmod{i}"] = np.ascontiguousarray(wmod_t[4 * i:4 * i + 4])
        in_maps.append(m)

    if _PREP_ONLY:
        return in_maps
    if _PROGRAM is None:
        _PROGRAM = build_program()
    res = run_bass_kernel_spmd(_PROGRAM, in_maps, core_ids=list(range(NCORES)))
    return _assemble([res.results[core]["yout"] for core in range(NCORES)])


def _assemble(ys):
    out = np.zeros((4, SEQ, D), np.float32)
    for core in range(NCORES):
        b, half = core // 2, core % 2
        y = ys[core]
        if half == 0:
            out[b, 0:HALF] = y
        else:
            out[b, HALF:SEQ] = y[::-1]
    return out
```
